# Optimizing a Trainium2 kernel written in Bass

```python
import jax, jax.numpy as jnp
from jax import lax
import numpy as np


D_MODEL = 1024
BATCH = 8
SEQ = 4096
DEPTH = 4

CTX_LEN = 256
GRID_W = 64
EPS = 1e-6
MIN_FORGET = 1e-20

D_MIX = D_MODEL
HG_HEADS = 4
HG_DIM = 64
HG_WIDTH = HG_HEADS * HG_DIM
HG_CHUNK = 64
MLA_HEADS = 6
MLA_NOPE = 64
MLA_ROPE = 32
MLA_V = 64
MLA_Q_RANK = 256
MLA_KV_RANK = 128
MLA_WIDTH = MLA_HEADS * MLA_V
NA_HEADS = 6
NA_DIM = 64
NA_WIDTH = NA_HEADS * NA_DIM
NA_WIN_R = 8
NA_WIN_C = 16
HG_IN = 5 * HG_WIDTH
MLA_IN = MLA_Q_RANK + MLA_KV_RANK + MLA_ROPE
NA_IN = 3 * NA_WIDTH
D_IN = HG_IN + MLA_IN + NA_IN
ROPE_BASE = 10000.0
Q_BLOCK = 128
N_EXPERTS = 16
EC_CAPACITY = 2
F_EXPERT = 1024

kernel_name = 'hybrid_hgrn2_mla_natten_ec_dit'


def _rmsnorm(x, g):
    x32 = x.astype(jnp.float32)
    y = x32 * lax.rsqrt(jnp.mean(x32 * x32, axis=-1, keepdims=True) + EPS)
    return (y * g.astype(jnp.float32)).astype(x.dtype)


def _split_heads(a, n_heads):
    B, L, W = a.shape
    return a.reshape(B, L, n_heads, W // n_heads).transpose(0, 2, 1, 3)


def _merge_heads(a):
    B, H, L, e = a.shape
    return a.transpose(0, 2, 1, 3).reshape(B, L, H * e)


def _rope_2d(x, n):
    t = jnp.arange(n)
    half = MLA_ROPE // 2
    inv = ROPE_BASE ** (-jnp.arange(0, half, 2, dtype=jnp.float32) / half)

    def rot(xa, pos):
        ang = pos.astype(jnp.float32)[:, None] * inv
        shape = (n,) + (1,) * (xa.ndim - 3) + (half // 2,)
        cos = jnp.cos(ang).reshape(shape).astype(xa.dtype)
        sin = jnp.sin(ang).reshape(shape).astype(xa.dtype)
        x1, x2 = xa[..., :half // 2], xa[..., half // 2:]
        return jnp.concatenate([x1 * cos - x2 * sin, x2 * cos + x1 * sin], axis=-1)

    return jnp.concatenate([rot(x[..., :half], t // GRID_W), rot(x[..., half:], t % GRID_W)], axis=-1)


def _softmax_attend(q, k, v, scale):
    s = jnp.einsum('bhqd,bhkd->bhqk', q, k).astype(jnp.float32) * scale
    p = jax.nn.softmax(s, axis=-1).astype(v.dtype)
    return jnp.einsum('bhqk,bhke->bhqe', p, v)


def _blocked_attend(q, k, v, scale):
    B, H, N, d = q.shape
    nb = N // Q_BLOCK
    qb = q.reshape(B, H, nb, Q_BLOCK, d).transpose(2, 0, 1, 3, 4)
    o = lax.map(lambda qq: _softmax_attend(qq, k, v, scale), qb)
    return o.transpose(1, 2, 0, 3, 4).reshape(B, H, N, v.shape[-1])


def _log_forget(z, lb):
    lb = lb.reshape(HG_HEADS, 1, HG_DIM).astype(jnp.float32)
    f = lb + (1.0 - lb) * jax.nn.sigmoid(z.astype(jnp.float32))
    return jnp.log(jnp.maximum(f, MIN_FORGET))


def _gla_chunk_scan(q, k, v, logf, s0):
    B, H, L, dk = q.shape
    dv = v.shape[-1]
    nc = L // HG_CHUNK

    def chunks(a):
        return a.reshape(B, H, nc, HG_CHUNK, a.shape[-1]).transpose(2, 0, 1, 3, 4)

    lower = jnp.tril(jnp.ones((HG_CHUNK, HG_CHUNK), dtype=bool))[..., None]

    def step(S, inp):
        qc, kc, vc, lf = inp
        b = jnp.cumsum(lf, axis=-2)
        diff = b[..., :, None, :] - b[..., None, :, :]
        decay = jnp.where(lower, jnp.exp(jnp.minimum(diff, 0.0)), 0.0).astype(qc.dtype)
        a = jnp.einsum('bhid,bhjd,bhijd->bhij', qc, kc, decay)
        o = (jnp.einsum('bhij,bhje->bhie', a, vc)
             + jnp.einsum('bhid,bhde->bhie', qc * jnp.exp(b).astype(qc.dtype), S))
        b_last = b[..., -1:, :]
        S = (S * jnp.exp(b_last)[..., 0, :, None].astype(S.dtype)
             + jnp.einsum('bhjd,bhje->bhde', kc * jnp.exp(b_last - b).astype(kc.dtype), vc))
        return S, o

    S, o = lax.scan(step, s0, (chunks(q), chunks(k), chunks(v), chunks(logf)))
    return o.transpose(1, 2, 0, 3, 4).reshape(B, H, L, dv), S


def _gla_final_state(k, v, logf):
    rest = lax.cumsum(logf, axis=2, reverse=True) - logf
    return jnp.einsum('bhjd,bhje->bhde', k * jnp.exp(jnp.minimum(rest, 0.0)).astype(k.dtype), v)


def _hgrn2_mixer(p_lat, p_ctx, lb, onorm_g, with_ctx_out):
    def prep(p):
        q, i, zf, zb, g = jnp.split(p, 5, axis=-1)
        q = _split_heads(q, HG_HEADS) * (HG_DIM ** -0.5)
        lf = (_log_forget(_split_heads(zf, HG_HEADS), lb[0]),
              _log_forget(_split_heads(zb, HG_HEADS), lb[1]))
        ks = (-jnp.expm1(lf[0]).astype(q.dtype), -jnp.expm1(lf[1]).astype(q.dtype))
        return q, _split_heads(i, HG_HEADS), lf, ks, g

    def flip(a):
        return jnp.flip(a, axis=2)

    def readout(o, g):
        return _merge_heads(_rmsnorm(o, onorm_g)) * jax.nn.silu(g)

    qc, ic, lfc, kc, gc = prep(p_ctx)
    ql, il, lfl, kl, gl = prep(p_lat)
    if with_ctx_out:
        zeros = jnp.zeros(qc.shape[:2] + (HG_DIM, HG_DIM), qc.dtype)
        oc_f, s_f = _gla_chunk_scan(qc, kc[0], ic, lfc[0], zeros)
        oc_b, s_b = _gla_chunk_scan(flip(qc), flip(kc[1]), flip(ic), flip(lfc[1]), zeros)
        o_ctx = readout(oc_f + flip(oc_b), gc)
    else:
        s_f = _gla_final_state(kc[0], ic, lfc[0])
        s_b = _gla_final_state(flip(kc[1]), flip(ic), flip(lfc[1]))
        o_ctx = None
    ol_f, _ = _gla_chunk_scan(ql, kl[0], il, lfl[0], s_f)
    ol_b, _ = _gla_chunk_scan(flip(ql), flip(kl[1]), flip(il), flip(lfl[1]), s_b)
    return readout(ol_f + flip(ol_b), gl), o_ctx


def _mla_mixer(p_lat, p_ctx, qnorm_g, w_uq, kvnorm_g, w_ukv, with_ctx_out):
    def project(p, rotate):
        B, L, _ = p.shape
        cq = p[..., :MLA_Q_RANK]
        ckv = p[..., MLA_Q_RANK:MLA_Q_RANK + MLA_KV_RANK]
        kr = p[..., MLA_Q_RANK + MLA_KV_RANK:]
        q = (_rmsnorm(cq, qnorm_g) @ w_uq).reshape(B, L, MLA_HEADS, MLA_NOPE + MLA_ROPE)
        kv = (_rmsnorm(ckv, kvnorm_g) @ w_ukv).reshape(B, L, MLA_HEADS, MLA_NOPE + MLA_V)
        q_nope, q_rope = q[..., :MLA_NOPE], q[..., MLA_NOPE:]
        k_nope, v = kv[..., :MLA_NOPE], kv[..., MLA_NOPE:]
        if rotate:
            q_rope = _rope_2d(q_rope, L)
            kr = _rope_2d(kr, L)
        k_rope = jnp.broadcast_to(kr[:, :, None, :], (B, L, MLA_HEADS, MLA_ROPE))
        q = jnp.concatenate([q_nope, q_rope], axis=-1).transpose(0, 2, 1, 3)
        k = jnp.concatenate([k_nope, k_rope], axis=-1).transpose(0, 2, 1, 3)
        return q, k, v.transpose(0, 2, 1, 3)

    scale = (MLA_NOPE + MLA_ROPE) ** -0.5
    ql, kl, vl = project(p_lat, True)
    qc, kc, vc = project(p_ctx, False)
    o_lat = _merge_heads(_blocked_attend(ql, jnp.concatenate([kl, kc], axis=2),
                                         jnp.concatenate([vl, vc], axis=2), scale))
    o_ctx = _merge_heads(_softmax_attend(qc, kc, vc, scale)) if with_ctx_out else None
    return o_lat, o_ctx


def _na_mixer(p_lat, p_ctx, rpb, with_ctx_out):
    ql, kl, vl = [_split_heads(a, NA_HEADS) for a in jnp.split(p_lat, 3, axis=-1)]
    qc, kc, vc = [_split_heads(a, NA_HEADS) for a in jnp.split(p_ctx, 3, axis=-1)]
    scale = NA_DIM ** -0.5
    B, H, N, d = ql.shape
    rows = N // GRID_W
    wr = min(NA_WIN_R, rows)
    kg = kl.reshape(B, H, rows, GRID_W, d)
    vg = vl.reshape(B, H, rows, GRID_W, d)
    cols = jnp.arange(GRID_W)
    col_idx = jnp.clip(cols - NA_WIN_C // 2, 0, GRID_W - NA_WIN_C)[:, None] + jnp.arange(NA_WIN_C)
    dcol = col_idx - cols[:, None] + (NA_WIN_C - 1)
    nwin = wr * NA_WIN_C

    def row_step(inp):
        r, q_r = inp
        rs = jnp.clip(r - wr // 2, 0, rows - wr)
        k_win = lax.dynamic_slice_in_dim(kg, rs, wr, axis=2)[:, :, :, col_idx]
        v_win = lax.dynamic_slice_in_dim(vg, rs, wr, axis=2)[:, :, :, col_idx]
        drow = rs + jnp.arange(wr) - r + (NA_WIN_R - 1)
        bias = rpb[:, drow[None, :, None], dcol[:, None, :]]
        s_win = (jnp.einsum('bhcd,bhrckd->bhcrk', q_r, k_win).astype(jnp.float32) * scale
                 + bias.astype(jnp.float32)).reshape(B, H, GRID_W, nwin)
        s_ctx = jnp.einsum('bhcd,bhjd->bhcj', q_r, kc).astype(jnp.float32) * scale
        p = jax.nn.softmax(jnp.concatenate([s_win, s_ctx], axis=-1), axis=-1).astype(vc.dtype)
        p_win = p[..., :nwin].reshape(B, H, GRID_W, wr, NA_WIN_C)
        return (jnp.einsum('bhcrk,bhrckd->bhcd', p_win, v_win)
                + jnp.einsum('bhcj,bhjd->bhcd', p[..., nwin:], vc))

    q_rows = ql.reshape(B, H, rows, GRID_W, d).transpose(2, 0, 1, 3, 4)
    o = lax.map(row_step, (jnp.arange(rows), q_rows))
    o_lat = o.transpose(1, 0, 3, 2, 4).reshape(B, N, H * d)
    o_ctx = _merge_heads(_softmax_attend(qc, kc, vc, scale)) if with_ctx_out else None
    return o_lat, o_ctx


def _ec_ffn(h, router_w, w1, w3, w2):
    B, L, D = h.shape
    cap = EC_CAPACITY * L // N_EXPERTS
    aff = jax.nn.softmax((h @ router_w).astype(jnp.float32), axis=-1)
    gate, idx = lax.top_k(aff.transpose(0, 2, 1), cap)
    xs = jax.vmap(lambda hb, ib: hb[ib])(h, idx)
    a = jnp.einsum('becd,edf->becf', xs, w1)
    u = jnp.einsum('becd,edf->becf', xs, w3)
    y = jnp.einsum('becf,efd->becd', jax.nn.silu(a) * u, w2) * gate[..., None].astype(h.dtype)
    return jax.vmap(lambda yb, ib: jnp.zeros((L, D), h.dtype).at[ib.reshape(-1)].add(yb.reshape(-1, D)))(y, idx)


def setup_inputs(seed: int = 0) -> dict:
    key = jax.random.key(seed)
    ks = jax.random.split(key, 22)

    def nrm(k, shape, scale):
        return jax.random.normal(k, shape, jnp.float32) * scale

    return {
        'x': nrm(ks[0], (BATCH, SEQ, D_MODEL), 1.0),
        'c': nrm(ks[1], (BATCH, D_MODEL), 1.0),
        'ctx': nrm(ks[2], (BATCH, CTX_LEN, D_MODEL), 1.0),
        'c_ctx': nrm(ks[3], (D_MODEL,), 1.0),
        'ada_w': nrm(ks[4], (DEPTH, D_MODEL, 6 * D_MODEL), 0.5 * D_MODEL ** -0.5),
        'ada_b': nrm(ks[5], (DEPTH, 6 * D_MODEL), 0.02),
        'norm_mix_g': 1.0 + nrm(ks[6], (DEPTH, D_MODEL), 0.02),
        'norm_ffn_g': 1.0 + nrm(ks[7], (DEPTH, D_MODEL), 0.02),
        'w_in': nrm(ks[8], (DEPTH, D_MODEL, D_IN), D_MODEL ** -0.5),
        'hgrn_lb_logits': nrm(ks[9], (2, DEPTH, HG_WIDTH), 0.1),
        'hgrn_onorm_g': 1.0 + nrm(ks[10], (DEPTH, HG_DIM), 0.02),
        'mla_qnorm_g': 1.0 + nrm(ks[11], (DEPTH, MLA_Q_RANK), 0.02),
        'mla_w_uq': nrm(ks[12], (DEPTH, MLA_Q_RANK, MLA_HEADS * (MLA_NOPE + MLA_ROPE)), MLA_Q_RANK ** -0.5),
        'mla_kvnorm_g': 1.0 + nrm(ks[13], (DEPTH, MLA_KV_RANK), 0.02),
        'mla_w_ukv': nrm(ks[14], (DEPTH, MLA_KV_RANK, MLA_HEADS * (MLA_NOPE + MLA_V)), MLA_KV_RANK ** -0.5),
        'na_rpb': nrm(ks[15], (DEPTH, NA_HEADS, 2 * NA_WIN_R - 1, 2 * NA_WIN_C - 1), 0.1),
        'w_out': nrm(ks[16], (DEPTH, D_MIX, D_MODEL), D_MIX ** -0.5),
        'router_w': nrm(ks[17], (DEPTH, D_MODEL, N_EXPERTS), D_MODEL ** -0.5),
        'exp_w1': nrm(ks[18], (DEPTH, N_EXPERTS, D_MODEL, F_EXPERT), D_MODEL ** -0.5),
        'exp_w3': nrm(ks[19], (DEPTH, N_EXPERTS, D_MODEL, F_EXPERT), D_MODEL ** -0.5),
        'exp_w2': nrm(ks[20], (DEPTH, N_EXPERTS, F_EXPERT, D_MODEL), F_EXPERT ** -0.5),
        'final_norm_g': 1.0 + nrm(ks[21], (D_MODEL,), 0.02),
    }


def reference(x, c, ctx, c_ctx, ada_w, ada_b, norm_mix_g, norm_ffn_g, w_in, hgrn_lb_logits,
              hgrn_onorm_g, mla_qnorm_g, mla_w_uq, mla_kvnorm_g, mla_w_ukv, na_rpb, w_out,
              router_w, exp_w1, exp_w3, exp_w2, final_norm_g):
    lb_w = jax.nn.softmax(hgrn_lb_logits.astype(jnp.float32), axis=1)
    lb_all = jnp.cumsum(lb_w, axis=1) - lb_w[:, :1]
    s_lat = jax.nn.silu(c)
    s_ctx = jax.nn.silu(c_ctx)
    for layer in range(DEPTH):
        with_ctx_out = layer < DEPTH - 1
        mod_l = (s_lat @ ada_w[layer] + ada_b[layer])[:, None, :]
        mod_c = s_ctx @ ada_w[layer] + ada_b[layer]
        sh_a, sc_a, g_a, sh_f, sc_f, g_f = jnp.split(mod_l, 6, axis=-1)
        csh_a, csc_a, cg_a, csh_f, csc_f, cg_f = jnp.split(mod_c, 6, axis=-1)

        h_l = _rmsnorm(x, norm_mix_g[layer]) * (1.0 + sc_a) + sh_a
        h_c = _rmsnorm(ctx, norm_mix_g[layer]) * (1.0 + csc_a) + csh_a
        p_l = h_l @ w_in[layer]
        p_c = h_c @ w_in[layer]
        a0, a1 = HG_IN, HG_IN + MLA_IN
        hg_l, hg_c = _hgrn2_mixer(p_l[..., :a0], p_c[..., :a0], lb_all[:, layer],
                                  hgrn_onorm_g[layer], with_ctx_out)
        ml_l, ml_c = _mla_mixer(p_l[..., a0:a1], p_c[..., a0:a1], mla_qnorm_g[layer], mla_w_uq[layer],
                                mla_kvnorm_g[layer], mla_w_ukv[layer], with_ctx_out)
        na_l, na_c = _na_mixer(p_l[..., a1:], p_c[..., a1:], na_rpb[layer], with_ctx_out)
        x = x + g_a * (jnp.concatenate([hg_l, ml_l, na_l], axis=-1) @ w_out[layer])

        h = _rmsnorm(x, norm_ffn_g[layer]) * (1.0 + sc_f) + sh_f
        x = x + g_f * _ec_ffn(h, router_w[layer], exp_w1[layer], exp_w3[layer], exp_w2[layer])

        if with_ctx_out:
            ctx = ctx + cg_a * (jnp.concatenate([hg_c, ml_c, na_c], axis=-1) @ w_out[layer])
            hc = _rmsnorm(ctx, norm_ffn_g[layer]) * (1.0 + csc_f) + csh_f
            ctx = ctx + cg_f * _ec_ffn(hc, router_w[layer], exp_w1[layer], exp_w3[layer], exp_w2[layer])
    return _rmsnorm(x, final_norm_g)
```

```python
from contextlib import ExitStack
import numpy as np
import ml_dtypes
import concourse.bass as bass
import concourse.mybir as mybir
from concourse.bass_utils import run_bass_kernel_spmd

F32 = mybir.dt.float32
BF16 = mybir.dt.bfloat16
I32 = mybir.dt.int32
U8 = mybir.dt.uint8
AF = mybir.ActivationFunctionType
ALU = mybir.AluOpType
AX = mybir.AxisListType

T = 4096
C = 256
TT = T + C
NTILE = TT // 128
D = 1024
DIN = 2848
DEPTH = 4
CHUNKS = [(i * 512, 512) for i in range(8)] + [(4096, 256)]


class Op:
    __slots__ = ("eng", "fn", "deps", "dma", "ndma", "signal", "count", "sem", "semval", "prevwait")

    def __init__(self, eng, fn, deps, dma, ndma):
        self.eng, self.fn, self.deps, self.dma, self.ndma = eng, fn, deps, dma, ndma
        self.signal = False
        self.count = 0
        self.sem = None
        self.semval = 0
        self.prevwait = None


class Sched:
    ENGS = ("pe", "act", "dve", "pool", "sp")
    NO_SELF_SYNC = ("pe", "sp")

    def __init__(self, nc, n_dma_sems=48):
        self.nc = nc
        self.ops = []
        self.lastw = {}
        self.readers = {}
        self.n_dma_sems = n_dma_sems

    def add(self, eng, fn, reads=(), writes=(), dma=False, ndma=1):
        idx = len(self.ops)
        deps = set()
        for k in reads:
            w = self.lastw.get(k)
            if w is not None:
                deps.add(w)
        for k in writes:
            w = self.lastw.get(k)
            if w is not None:
                deps.add(w)
            deps.update(self.readers.get(k, ()))
        self.ops.append(Op(eng, fn, sorted(deps), dma, ndma))
        for k in reads:
            self.readers.setdefault(k, []).append(idx)
        for k in writes:
            self.lastw[k] = idx
            self.readers[k] = []
        return idx

    def barrier(self):
        keys = list(set(self.lastw.keys()) | set(self.readers.keys()))
        self.add("pool", lambda e: e.nop(), writes=keys + ["__bar"])
        for en in self.ENGS:
            if en != "pool":
                self.add(en, lambda e: e.nop(), reads=["__bar"])

    def emit(self, final_wait_ops=()):
        nc, ops = self.nc, self.ops
        EPOCH = 30000
        for op in ops:
            for d in op.deps:
                dop = ops[d]
                if dop.dma:
                    continue
                if dop.eng == op.eng and dop.eng in self.NO_SELF_SYNC and not op.dma:
                    continue
                dop.signal = True
        for d in final_wait_ops:
            if not ops[d].dma:
                ops[d].signal = True
        cnt = {e: 0 for e in self.ENGS}
        for op in ops:
            if op.dma:
                continue
            if op.signal:
                cnt[op.eng] += 1
            op.count = cnt[op.eng]
        n_epochs = {e: max(0, cnt[e] - 1) // EPOCH + 1 for e in self.ENGS}
        with ExitStack() as es:
            esem = {e: [es.enter_context(nc.semaphore(f"s_{e}{i}")) for i in range(n_epochs[e])] for e in self.ENGS}
            dsem = [es.enter_context(nc.semaphore(f"s_dma{i}")) for i in range(self.n_dma_sems)]
            dval = [0] * self.n_dma_sems
            k = 0
            for op in ops:
                if op.dma:
                    s = k % self.n_dma_sems
                    k += 1
                    op.sem = s
                    op.prevwait = dval[s] if dval[s] > 0 else None
                    dval[s] += 16 * op.ndma
                    op.semval = dval[s]
            per_eng = {e: [] for e in self.ENGS}
            for i, op in enumerate(ops):
                per_eng[op.eng].append(i)

            def run(ename, eng):
                seen = {}

                def wait(sem, val):
                    if seen.get(sem.num, -1) >= val:
                        return
                    seen[sem.num] = val
                    eng.wait_ge(sem, val)

                def wait_op(dop):
                    if dop.dma:
                        wait(dsem[dop.sem], dop.semval)
                    else:
                        ep = (dop.count - 1) // EPOCH
                        wait(esem[dop.eng][ep], dop.count - ep * EPOCH)

                for i in per_eng[ename]:
                    op = ops[i]
                    for d in op.deps:
                        dop = ops[d]
                        if (not dop.dma) and dop.eng == ename and ename in self.NO_SELF_SYNC and not op.dma:
                            continue
                        wait_op(dop)
                    if op.dma:
                        if op.prevwait is not None:
                            wait(dsem[op.sem], op.prevwait)
                        op.fn(eng, dsem[op.sem])
                    else:
                        ins = op.fn(eng)
                        if op.signal:
                            ep = (op.count - 1) // EPOCH
                            ins.then_inc(esem[ename][ep], 1)
                if ename == "sp":
                    for d in final_wait_ops:
                        wait_op(ops[d])

            with nc.Block() as block:

                @block.sync
                def _(e):
                    run("sp", e)

                @block.scalar
                def _(e):
                    run("act", e)

                @block.vector
                def _(e):
                    run("dve", e)

                @block.gpsimd
                def _(e):
                    run("pool", e)

                @block.tensor
                def _(e):
                    run("pe", e)


DTSIZE = {F32: 4, BF16: 2, I32: 4, U8: 1}


class Arena:
    def __init__(self, ap, nbytes, name):
        self.ap, self.nbytes, self.off, self.name = ap, nbytes, 0, name
        self.n = 0

    def reset(self):
        self.off = 0

    def alloc(self, shape, dt, parts=128):
        free = 1
        for s in shape:
            free *= s
        nb = free * DTSIZE[dt]
        nb_al = (nb + 63) // 64 * 64
        assert self.off + nb_al <= self.nbytes, (self.name, self.off, nb_al, self.nbytes)
        v = self.ap[0:parts, self.off:self.off + nb].bitcast(dt)
        self.off += nb_al
        self.n += 1
        if len(shape) > 1:
            names = [chr(ord("a") + i) for i in range(len(shape))]
            kw = {n: int(sz) for n, sz in zip(names[:-1], shape[:-1])}
            v = v.rearrange("p (" + " ".join(names) + ") -> p " + " ".join(names), **kw)
        return v


def build(n_layers=DEPTH, debug=(), stop=None, only=None):
    nc = bass.Bass("TRN2", target_bir_lowering=False)
    S = Sched(nc)

    def din(name, shape, dt=F32):
        if only is not None and name in ("ada_w", "w_in", "w_uq", "w_ukv", "nab", "w_out", "router_w", "exp_w1", "exp_w3", "exp_w2"):
            shape = [1] * len(shape)
        return nc.dram_tensor(name, list(shape), dt, kind="ExternalInput").ap()

    def dscr(name, shape, dt=F32):
        kind = "ExternalOutput" if name in debug else "Internal"
        return nc.dram_tensor(name, list(shape), dt, kind=kind).ap()

    x_in = din("x", [T, D])
    ctx_in = din("ctx", [C, D])
    ccol = din("ccol", [128, 8, 2])
    ada_w = din("ada_w", [DEPTH, D, 6 * D])
    ada_bT = din("ada_bT", [DEPTH, 128, 48])
    gmixT = din("gmixT", [DEPTH, 128, 8])
    gffnT = din("gffnT", [DEPTH, 128, 8])
    w_in = din("w_in", [DEPTH, D, DIN + 32])
    lbl = din("lbl", [128, 2, DEPTH, 256])
    onormg = din("onormg", [DEPTH, 128, 256])
    qng = din("qng", [DEPTH, 128, 2])
    kvng = din("kvng", [DEPTH, 128, 1])
    w_uq = din("w_uq", [DEPTH, 256, 1152])
    w_ukv = din("w_ukv", [DEPTH, 128, 768])
    rope = din("rope", [2, 32, TT])
    nab = din("nab", [DEPTH, 6, 128, 1536])
    rowmask = din("rowmask", [128, 20, 512], BF16)
    w_out = din("w_out", [DEPTH, D, D])
    router_w = din("router_w", [DEPTH, D, 16])
    exp_w1 = din("exp_w1", [DEPTH, 16, D, D])
    exp_w3 = din("exp_w3", [DEPTH, 16, D, D])
    exp_w2 = din("exp_w2", [DEPTH, 16, D, D])
    fng = din("fng", [128, D])
    ident_d = din("ident", [128, 128], BF16)
    identf_d = din("identf", [128, 128])
    consts_d = din("consts", [128, 5, 128])
    sel4_d = din("sel4", [128, 16])
    vmask_d = din("vmask", [128, 4])
    tokinfo_d = din("tokinfo", [128, NTILE, 2])
    iota_d = din("iota", [128, 128])
    out = nc.dram_tensor("out", [T, D], F32, kind="ExternalOutput").ap()

    XS_h = nc.dram_tensor("XS", [TT + 128, D], F32, kind=("ExternalOutput" if "XS" in debug else "Internal"))
    XS = XS_h.ap()
    PHG = dscr("PHG", [TT, 1280]) if only != "E" else din("PHG", [TT, 1280])
    NAQ = dscr("NAQ", [384, TT], BF16)
    NAK = dscr("NAK", [384, TT], BF16)
    NAV = dscr("NAV", [6, 128, NTILE, 65], BF16)
    MQ = dscr("MQ", [6, 96, TT], BF16)
    MK = dscr("MK", [6, 96, TT], BF16)
    MV = dscr("MV", [6, 128, NTILE, 65], BF16)
    CAT = dscr("CAT", [16, 64, TT], BF16)
    OF = dscr("OF", [TT, 256])
    H2 = dscr("H2", [TT, D], BF16)

    final_ops = []
    es = ExitStack()
    with es:
        sbuf_all = es.enter_context(nc.sbuf_tensor("arena", [128, 195 * 1024], U8))
        persist_ap = sbuf_all[:, 0:24 * 1024]
        PA = Arena(sbuf_all[:, 0:24 * 1024], 24 * 1024, "persist")
        WA = Arena(sbuf_all[:, 24 * 1024:195 * 1024], 171 * 1024, "work")
        yo_t = [es.enter_context(nc.sbuf_tensor(f"yo{i}", [128, D], F32)) for i in range(2)]
        idx_t = [es.enter_context(nc.sbuf_tensor(f"idxi{i}", [128, 9, 1], I32)) for i in range(2)]
        ps = [es.enter_context(nc.psum_tensor(f"ps{i}", [128, 512], F32)) for i in range(8)]
        psn = [0]

        def P():
            i = psn[0] % 6
            psn[0] += 1
            return ps[i][:], f"ps{i}"

        def dma(q, o, i, reads=(), writes=(), **kw):
            return S.add(q, lambda e, s: e.dma_start(out=o, in_=i, **kw).then_inc(s, 16), reads, writes, dma=True)

        def op(eng, meth, reads, writes, *a, **kw):
            return S.add(eng, lambda e: getattr(e, meth)(*a, **kw), reads, writes)

        regs = {}

        def _mk_bc(e):
            regs["bc"] = e.alloc_register("bc")
            return e.reg_mov(regs["bc"], TT + 127)

        S.add("pool", _mk_bc)
        ident = PA.alloc([128], BF16)
        identf = PA.alloc([128], F32)
        onesb = PA.alloc([128], BF16)
        onesf = PA.alloc([128], F32)
        epsb = PA.alloc([1], F32)
        consts = PA.alloc([5, 128], F32)
        maskb = PA.alloc([1, 128], BF16)
        sel4 = PA.alloc([16], F32)
        vmask = PA.alloc([4], F32)
        MOD = PA.alloc([DEPTH, 48, 2], F32)
        AB = PA.alloc([DEPTH, 4, 8, 2], F32)
        LB = PA.alloc([2, DEPTH, 256], F32)
        OML = PA.alloc([2, DEPTH, 256], F32)
        dma("sp", ident, ident_d, writes=["ident"])
        dma("sp", identf, identf_d, writes=["identf"])
        dma("sp", consts, consts_d, writes=["consts"])
        dma("sp", sel4, sel4_d, writes=["sel4"])
        dma("sp", vmask, vmask_d, writes=["vmask"])
        op("dve", "memset", [], ["onesb"], onesb, 1.0)
        op("dve", "memset", [], ["onesf"], onesf, 1.0)
        op("dve", "memset", [], ["epsb"], epsb, 1e-6)
        op("dve", "tensor_copy", ["consts"], ["maskb"], out=maskb, in_=consts[:, 4:5, :])

        for t_ in range(32):
            dma("sp", XS[t_ * 128:(t_ + 1) * 128, :], x_in[t_ * 128:(t_ + 1) * 128, :], writes=[("XSi", t_)])
        for t_ in range(2):
            dma("sp", XS[T + t_ * 128:T + (t_ + 1) * 128, :], ctx_in[t_ * 128:(t_ + 1) * 128, :], writes=[("XSi", 32 + t_)])

        WA.reset()
        lraw = WA.alloc([2, DEPTH, 256], F32)
        lsum = WA.alloc([2, 256], F32)
        dma("sp", lraw, lbl, writes=["lraw"])
        op("act", "activation", ["lraw"], ["lraw"], out=lraw, in_=lraw, func=AF.Exp)
        op("dve", "tensor_tensor", ["lraw"], ["lsum"], out=lsum, in0=lraw[:, :, 0, :], in1=lraw[:, :, 1, :], op=ALU.add)
        op("dve", "tensor_tensor", ["lraw", "lsum"], ["lsum"], out=lsum, in0=lsum, in1=lraw[:, :, 2, :], op=ALU.add)
        op("dve", "tensor_tensor", ["lraw", "lsum"], ["lsum"], out=lsum, in0=lsum, in1=lraw[:, :, 3, :], op=ALU.add)
        op("dve", "reciprocal", ["lsum"], ["lsum"], out=lsum, in_=lsum)
        for l in range(DEPTH):
            op("dve", "tensor_tensor", ["lraw", "lsum"], ["lraw"], out=lraw[:, :, l, :], in0=lraw[:, :, l, :], in1=lsum, op=ALU.mult)
        op("dve", "memset", [], ["LB"], LB[:, :, 0, :], 0.0)
        for l in range(1, DEPTH):
            op("dve", "tensor_tensor", ["lraw", "LB"], ["LB"], out=LB[:, :, l, :], in0=LB[:, :, l - 1, :], in1=lraw[:, :, l, :], op=ALU.add)
        op("dve", "tensor_scalar", ["LB"], ["OML"], out=OML, in0=LB, scalar1=-1.0, scalar2=1.0, op0=ALU.mult, op1=ALU.add)
        if only is not None:
            S.barrier()

        if only is None:
            ccs = WA.alloc([8, 2], F32)
            scol = WA.alloc([8, 2], BF16)
            tmpc = WA.alloc([8, 2], F32)
            adab = WA.alloc([DEPTH, 48], F32)
            gmx = WA.alloc([DEPTH, 8], F32)
            gff = WA.alloc([DEPTH, 8], F32)
            dma("sp", ccs, ccol, writes=["ccs"])
            dma("sp", adab, ada_bT.rearrange("l p j -> p l j"), writes=["adab"])
            dma("sp", gmx, gmixT.rearrange("l p j -> p l j"), writes=["gmx"])
            dma("sp", gff, gffnT.rearrange("l p j -> p l j"), writes=["gff"])
            op("act", "activation", ["ccs"], ["tmpc"], out=tmpc, in_=ccs, func=AF.Exp, scale=-1.0)
            op("dve", "tensor_scalar_add", ["tmpc"], ["tmpc"], out=tmpc, in0=tmpc, scalar1=1.0)
            op("dve", "reciprocal", ["tmpc"], ["tmpc"], out=tmpc, in_=tmpc)
            op("dve", "tensor_tensor", ["tmpc", "ccs"], ["scol"], out=scol, in0=tmpc, in1=ccs, op=ALU.mult)
            awb = [WA.alloc([8, 1536], BF16) for _ in range(2)]
            ci = 0
            for l in range(DEPTH):
                pt, pk = P()
                pv = pt[:, 0:96].rearrange("p (j w) -> p j w", w=2)
                for cc in range(4):
                    b = ci % 2
                    ci += 1
                    for kt_ in range(8):
                        dma("pool", awb[b][:, kt_, :], ada_w[l, kt_ * 128:(kt_ + 1) * 128, cc * 1536:(cc + 1) * 1536], writes=[f"awb{b}"])
                    for jj in range(12):
                        jt = cc * 12 + jj
                        for kt in range(8):
                            op("pe", "matmul", [f"awb{b}", "scol"], [pk], pv[:, jt, :], lhsT=awb[b][:, kt, jj * 128:(jj + 1) * 128],
                               rhs=scol[:, kt, :], start=(kt == 0), stop=(kt == 7))
                op("dve", "tensor_tensor", [pk, "adab"], ["MOD"], out=MOD[:, l], in0=pv,
                   in1=adab[:, l, :].unsqueeze(2).to_broadcast([128, 48, 2]), op=ALU.add)
                for (qi, sc_q, sh_q, g) in ((0, 1, 0, gmx), (2, 4, 3, gff)):
                    op("dve", "tensor_scalar_add", ["MOD"], ["AB"], out=AB[:, l, qi], in0=MOD[:, l, sc_q * 8:(sc_q + 1) * 8, :], scalar1=1.0)
                    op("dve", "tensor_tensor", ["AB", g is gmx and "gmx" or "gff"], ["AB"], out=AB[:, l, qi], in0=AB[:, l, qi],
                       in1=g[:, l, :].unsqueeze(2).to_broadcast([128, 8, 2]), op=ALU.mult)
                    op("dve", "tensor_copy", ["MOD"], ["AB"], out=AB[:, l, qi + 1], in_=MOD[:, l, sh_q * 8:(sh_q + 1) * 8, :])
            S.barrier()

        def norm_tile(xt, xk, l, qi, which, hT_dst, hT_key, scratch):
            sq, ssq, rstd, xn, tmp = scratch
            op("act", "activation", [xk], ["n_sq", "n_ssq"], out=sq, in_=xt, func=AF.Square, accum_out=ssq)
            op("act", "activation", ["n_ssq", "epsb"], ["n_rstd"], out=rstd, in_=ssq, func=AF.Ln, scale=1.0 / D, bias=epsb)
            op("act", "activation", ["n_rstd"], ["n_rstd"], out=rstd, in_=rstd, func=AF.Exp, scale=-0.5)
            op("act", "activation", [xk, "n_rstd"], ["n_xn"], out=xn, in_=xt, func=AF.Copy, scale=rstd)
            pt, pk = P()
            pb = pt[:].bitcast(BF16)
            for k in range(8):
                op("pe", "transpose", ["n_xn", "ident"], [pk], out=pb[:, k * 128:(k + 1) * 128], in_=xn[:, k * 128:(k + 1) * 128], identity=ident)
            pv = pb.rearrange("p (k n) -> p k n", k=8)
            op("dve", "tensor_tensor", [pk, "AB"], ["n_tmp"], out=tmp, in0=pv,
               in1=AB[:, l, qi, :, which].unsqueeze(2).to_broadcast([128, 8, 128]), op=ALU.mult)
            op("dve", "tensor_tensor", ["n_tmp", "AB"], [hT_key], out=hT_dst, in0=tmp,
               in1=AB[:, l, qi + 1, :, which].unsqueeze(2).to_broadcast([128, 8, 128]), op=ALU.add)

        acc_n = [0]
        for l in range(n_layers):
            with_ctx = l < DEPTH - 1
            if only is None:
                WA.reset()
                hT = WA.alloc([8, TT], BF16)
                winb = WA.alloc([8, DIN + 32], BF16)
                for kt_ in range(8):
                    dma("pool", winb[:, kt_, :], w_in[l, kt_ * 128:(kt_ + 1) * 128, :], writes=["winb"])
                markA = WA.off
                xts = [WA.alloc([D], F32) for _ in range(2)]
                scratch = (WA.alloc([D], F32), WA.alloc([1], F32), WA.alloc([1], F32), WA.alloc([D], BF16), WA.alloc([8, 128], F32))
                for t in range(NTILE):
                    b = t % 2
                    dma("sp", xts[b], XS[t * 128:(t + 1) * 128, :], reads=["XS"], writes=[f"xt{b}"])
                    norm_tile(xts[b], f"xt{b}", l, 0, 0 if t < 32 else 1, hT[:, :, t * 128:(t + 1) * 128], f"hT{t}", scratch)
                if stop == (l, "A"):
                    break
                S.barrier()
                WA.off = markA
                wuqb = WA.alloc([2, 1152], BF16)
                wukvb = WA.alloc([768], BF16)
                gq = WA.alloc([2], F32)
                gkv = WA.alloc([1], F32)
                for kt_ in range(2):
                    dma("pool", wuqb[:, kt_, :], w_uq[l, kt_ * 128:(kt_ + 1) * 128, :], writes=["wuqb"])
                dma("pool", wukvb, w_ukv[l], writes=["wukvb"])
                dma("sp", gq, qng[l], writes=["gq"])
                dma("sp", gkv, kvng[l], writes=["gkv"])
                ropq = [WA.alloc([2, 512], F32, parts=96)] * 2
                ropk = [WA.alloc([2, 512], F32, parts=32)] * 2
                cqb = WA.alloc([3, 512], BF16)
                sqb = WA.alloc([3, 512], BF16)
                Rq = WA.alloc([512], F32)
                Rk = WA.alloc([512], F32)
                cqn = WA.alloc([3, 512], BF16)
                t1 = WA.alloc([512], F32, parts=96)
                t2 = WA.alloc([512], F32, parts=96)
                qo = [WA.alloc([512], BF16, parts=96) for _ in range(2)]
                ko = [WA.alloc([512], BF16, parts=64) for _ in range(2)]
                kro = WA.alloc([512], BF16, parts=32)
                nqk = [WA.alloc([512], BF16) for _ in range(2)]
                phg = [WA.alloc([1280], F32) for _ in range(2)]
                vau = [WA.alloc([2, 6, 65], BF16) for _ in range(2)]
                for b in range(2):
                    op("dve", "memset", [], [f"vau{b}"], vau[b], 1.0)
                ev = [0]

                def evac(pk_, dst, dkey, src, extra_reads=(), scale=None):
                    ev[0] += 1
                    if ev[0] % 2 == 0:
                        if scale is None:
                            op("act", "activation", [pk_] + list(extra_reads), [dkey], out=dst, in_=src, func=AF.Copy)
                        else:
                            op("act", "activation", [pk_] + list(extra_reads), [dkey], out=dst, in_=src, func=AF.Copy, scale=scale)
                    else:
                        if scale is None:
                            op("dve", "tensor_copy", [pk_] + list(extra_reads), [dkey], out=dst, in_=src)
                        else:
                            op("dve", "tensor_scalar_mul", [pk_] + list(extra_reads), [dkey], out=dst, in0=src, scalar1=scale)

                for ci_, (c0, cn) in enumerate(CHUNKS):
                    cb = ci_ % 2
                    hkeys = [f"hT{t}" for t in range(c0 // 128, (c0 + cn) // 128)]
                    dma("sp", ropq[cb][64:96, :, 0:cn], rope[:, :, c0:c0 + cn].rearrange("w p n -> p w n"), writes=["ropq"])
                    dma("sp", ropk[cb][:, :, 0:cn], rope[:, :, c0:c0 + cn].rearrange("w p n -> p w n"), writes=["ropk"])

                    def proj_fm(col0, ncols, pt, pk):
                        for kt in range(8):
                            op("pe", "matmul", hkeys + ["winb"], [pk], pt[0:ncols, 0:cn], lhsT=winb[:, kt, col0:col0 + ncols],
                               rhs=hT[:, kt, c0:c0 + cn], start=(kt == 0), stop=(kt == 7))

                    for j in range(3):
                        pt, pk = P()
                        proj_fm(1280 + j * 128, 128, pt, pk)
                        op("act", "activation", [pk], ["cqb"], out=cqb[:, j, 0:cn], in_=pt[:, 0:cn], func=AF.Copy)
                        op("act", "activation", [pk], ["sqb"], out=sqb[:, j, 0:cn], in_=pt[:, 0:cn], func=AF.Square)
                    for (R_, rkey, js, nfeat) in ((Rq, "Rq", (0, 1), 256.0), (Rk, "Rk", (2,), 128.0)):
                        pt, pk = P()
                        for ii, j in enumerate(js):
                            op("pe", "matmul", ["sqb", "onesb"], [pk], pt[:, 0:cn], lhsT=onesb, rhs=sqb[:, j, 0:cn], start=(ii == 0), stop=(ii == len(js) - 1))
                        op("act", "activation", [pk, "epsb"], [rkey], out=R_[:, 0:cn], in_=pt[:, 0:cn], func=AF.Ln, scale=1.0 / nfeat, bias=epsb)
                        op("act", "activation", [rkey], [rkey], out=R_[:, 0:cn], in_=R_[:, 0:cn], func=AF.Exp, scale=-0.5)
                    for j in range(3):
                        R_, rkey, g_, gk = (Rq, "Rq", gq[:, j:j + 1], "gq") if j < 2 else (Rk, "Rk", gkv[:, 0:1], "gkv")
                        op("dve", "scalar_tensor_tensor", ["cqb", rkey, gk], ["cqn"], out=cqn[:, j, 0:cn], in0=cqb[:, j, 0:cn], scalar=g_,
                           in1=R_[:, 0:cn], op0=ALU.mult, op1=ALU.mult)
                    for h in range(6):
                        p1, k1 = P()
                        p2, k2 = P()
                        for (pp, kk, base) in ((p1, k1, 0), (p2, k2, 576)):
                            for j in range(2):
                                op("pe", "matmul", ["cqn", "wuqb"], [kk], pp[0:96, 0:cn], lhsT=wuqb[:, j, base + h * 96:base + (h + 1) * 96],
                                   rhs=cqn[:, j, 0:cn], start=(j == 0), stop=(j == 1))
                        qb_ = qo[h % 2]
                        qk_ = f"qo{h % 2}"
                        op("act", "activation", [k1], [qk_], out=qb_[0:64, 0:cn], in_=p1[0:64, 0:cn], func=AF.Copy)
                        op("dve", "tensor_tensor", [k1, "ropq"], ["t1"], out=t1[64:96, 0:cn], in0=p1[64:96, 0:cn], in1=ropq[cb][64:96, 0, 0:cn], op=ALU.mult)
                        op("dve", "tensor_tensor", [k2, "ropq"], ["t2"], out=t2[64:96, 0:cn], in0=p2[64:96, 0:cn], in1=ropq[cb][64:96, 1, 0:cn], op=ALU.mult)
                        op("dve", "tensor_tensor", ["t1", "t2", qk_], [qk_], out=qb_[64:96, 0:cn], in0=t1[64:96, 0:cn], in1=t2[64:96, 0:cn], op=ALU.add)
                        dma("sp", MQ[h, :, c0:c0 + cn], qb_[:, 0:cn], reads=[qk_], writes=["MQ"])
                    p1, k1 = P()
                    p2, k2 = P()
                    proj_fm(1664, 32, p1, k1)
                    proj_fm(DIN, 32, p2, k2)
                    op("dve", "tensor_tensor", [k1, "ropk"], ["t1"], out=t1[0:32, 0:cn], in0=p1[0:32, 0:cn], in1=ropk[cb][:, 0, 0:cn], op=ALU.mult)
                    op("dve", "tensor_tensor", [k2, "ropk"], ["t2"], out=t2[0:32, 0:cn], in0=p2[0:32, 0:cn], in1=ropk[cb][:, 1, 0:cn], op=ALU.mult)
                    op("dve", "tensor_tensor", ["t1", "t2"], ["kro"], out=kro[:, 0:cn], in0=t1[0:32, 0:cn], in1=t2[0:32, 0:cn], op=ALU.add)
                    for h in range(6):
                        dma("sp", MK[h, 64:96, c0:c0 + cn], kro[:, 0:cn], reads=["kro"], writes=["MK"])
                    for h in range(6):
                        pt, pk = P()
                        op("pe", "matmul", ["cqn", "wukvb"], [pk], pt[0:64, 0:cn], lhsT=wukvb[:, h * 128:h * 128 + 64], rhs=cqn[:, 2, 0:cn], start=True, stop=True)
                        kb_ = ko[h % 2]
                        evac(pk, kb_[:, 0:cn], f"ko{h % 2}", pt[0:64, 0:cn])
                        dma("sp", MK[h, 0:64, c0:c0 + cn], kb_[:, 0:cn], reads=[f"ko{h % 2}"], writes=["MK"])
                    for (col0, dst, sc) in ((1696, NAQ, 0.125), (2080, NAK, None)):
                        for j in range(3):
                            pt, pk = P()
                            proj_fm(col0 + j * 128, 128, pt, pk)
                            nb_ = nqk[j % 2]
                            evac(pk, nb_[:, 0:cn], f"nqk{j % 2}", pt[:, 0:cn], scale=sc)
                            dma("sp", dst[j * 128:(j + 1) * 128, c0:c0 + cn], nb_[:, 0:cn], reads=[f"nqk{j % 2}"], writes=["NAQK"])
                    for tt in range(c0 // 128, (c0 + cn) // 128):
                        tb = tt % 2
                        tl = (tt * 128 - c0)
                        for (col0, ncol) in ((0, 512), (512, 512), (1024, 256)):
                            pt, pk = P()
                            for kt in range(8):
                                op("pe", "matmul", [f"hT{tt}", "winb"], [pk], pt[:, 0:ncol], lhsT=hT[:, kt, tt * 128:(tt + 1) * 128],
                                   rhs=winb[:, kt, col0:col0 + ncol], start=(kt == 0), stop=(kt == 7))
                            evac(pk, phg[tb][:, col0:col0 + ncol], f"phg{tb}", pt[:, 0:ncol])
                        dma("sp", PHG[tt * 128:(tt + 1) * 128, :], phg[tb], reads=[f"phg{tb}"], writes=["PHG"])
                        pt, pk = P()
                        for kt in range(8):
                            op("pe", "matmul", [f"hT{tt}", "winb"], [pk], pt[:, 0:384], lhsT=hT[:, kt, tt * 128:(tt + 1) * 128],
                               rhs=winb[:, kt, 2464:2848], start=(kt == 0), stop=(kt == 7))
                        evac(pk, vau[tb][:, 0, :, 0:64], f"vau{tb}", pt[:, 0:384].rearrange("p (h e) -> p h e", h=6))
                        pt, pk = P()
                        wv = wukvb.rearrange("p (h e) -> p h e", h=6)[:, :, 64:128]
                        op("pe", "matmul", ["cqn", "wukvb"], [pk], pt[:, 0:384].rearrange("p (h e) -> p h e", h=6), lhsT=cqn[:, 2, tl:tl + 128], rhs=wv, start=True, stop=True)
                        evac(pk, vau[tb][:, 1, :, 0:64], f"vau{tb}", pt[:, 0:384].rearrange("p (h e) -> p h e", h=6))
                        [dma("sp", NAV[h3:h3 + 3, :, tt, :].rearrange("h p e -> p h e"), vau[tb][:, 0, h3:h3 + 3], reads=[f"vau{tb}"], writes=[("NAV", h3)]) for h3 in (0, 3)]
                        [dma("sp", MV[h3:h3 + 3, :, tt, :].rearrange("h p e -> p h e"), vau[tb][:, 1, h3:h3 + 3], reads=[f"vau{tb}"], writes=[("MV", h3)]) for h3 in (0, 3)]
                S.barrier()
                if stop == (l, "B"):
                    break

                def attention(slot, kdim, Ksrc, Qsrc, Vsrc, scale, plan, EBh=None):
                    hb = slot % 2
                    KTt, QTt, VAt = KT[hb], QT[hb], VA[hb]
                    dma("sp", KTt[0:kdim, :], Ksrc, writes=[f"KT{hb}"])
                    dma("sp", QTt[0:kdim, :], Qsrc, writes=[f"QT{hb}"])
                    dma("sp", VAt, Vsrc, writes=[f"VA{hb}"])
                    for ci_, (c0, cn, kts) in enumerate(plan):
                        ai = acc_n[0] % 2
                        acc_n[0] += 1
                        po, pok = ps[6 + ai][:], f"ps{6 + ai}"
                        for i, (kt, mi) in enumerate(kts):
                            pS, pSk = P()
                            op("pe", "matmul", [f"KT{hb}", f"QT{hb}"], [pSk], pS[:, 0:cn], lhsT=KTt[0:kdim, kt * 128:(kt + 1) * 128],
                               rhs=QTt[0:kdim, c0:c0 + cn], start=True, stop=True)
                            pi = pb_n[0] % 3
                            pb_n[0] += 1
                            op("act", "activation", [pSk], [f"pbuf{pi}"], out=pbuf[pi][:, 0:cn], in_=pS[:, 0:cn], func=AF.Exp, scale=scale)
                            src, sk = pbuf[pi], f"pbuf{pi}"
                            if mi is not None:
                                op("dve", "tensor_tensor", [sk, "EBh"], [f"pmsk{pi}"], out=pmsk[pi][:, 0:cn], in0=pbuf[pi][:, 0:cn], in1=EBh[:, mi, 0:cn], op=ALU.mult)
                                src, sk = pmsk[pi], f"pmsk{pi}"
                            op("pe", "matmul", [sk, f"VA{hb}"], [pok], po[0:65, 0:cn], lhsT=VAt[:, kt, :], rhs=src[:, 0:cn], start=(i == 0), stop=(i == len(kts) - 1))
                        op("act", "activation", [pok], ["rsum"], out=rsum[64:65, 0:cn], in_=po[64:65, 0:cn], func=AF.Copy)
                        op("dve", "reciprocal", ["rsum"], ["rsum"], out=rsum[64:65, 0:cn], in_=rsum[64:65, 0:cn])
                        pb_, pbk = P()
                        op("pe", "matmul", ["rsum", "onesf"], [pbk], pb_[0:64, 0:cn], lhsT=onesf[64:65, 0:64], rhs=rsum[64:65, 0:cn], start=True, stop=True)
                        op("act", "activation", [pok], ["osb"], out=osb[:, 0:cn], in_=po[0:64, 0:cn], func=AF.Copy)
                        oi = ob_n[0] % 2
                        ob_n[0] += 1
                        op("dve", "tensor_tensor", [pbk, "osb"], [f"otb{oi}"], out=otb[oi][:, 0:cn], in0=pb_[0:64, 0:cn], in1=osb[:, 0:cn], op=ALU.mult)
                        dma("sp", CAT[slot, :, c0:c0 + cn], otb[oi][:, 0:cn], reads=[f"otb{oi}"], writes=["CAT"])

                WA.reset()
                KT = [WA.alloc([TT], BF16, parts=96) for _ in range(2)]
                QT = [WA.alloc([TT], BF16, parts=96) for _ in range(2)]
                VA = [WA.alloc([NTILE, 65], BF16) for _ in range(2)]
                pbuf = [WA.alloc([512], BF16) for _ in range(3)]
                pmsk = [WA.alloc([512], BF16) for _ in range(3)]
                rsum = WA.alloc([512], F32, parts=65)
                osb = WA.alloc([512], F32, parts=64)
                otb = [WA.alloc([512], BF16, parts=64) for _ in range(2)]
                rmk = WA.alloc([20, 512], BF16)
                nraw = WA.alloc([1536], F32)
                cbu = WA.alloc([1536], BF16)
                EBh = WA.alloc([20, 512], BF16)
                pb_n, ob_n = [0], [0]
                dma("sp", rmk, rowmask, writes=["rmk"])
                allk = [(kt, None) for kt in range(NTILE)]
                ctxk = [(32, None), (33, None)]
                mla_plan = [(c0, cn, allk) for (c0, cn) in CHUNKS[:8]] + ([(T, C, ctxk)] if with_ctx else [])
                for h in range(6):
                    attention(4 + h, 96, MK[h], MQ[h], MV[h], 96.0 ** -0.5, mla_plan)
                na_plan = []
                for m in range(8):
                    kts = list(ctxk)
                    for o in range(8):
                        ktp = 4 * m - 2 + o
                        if 0 <= ktp <= 31:
                            ti = o if 1 <= m <= 6 else (8 + o - 2 if m == 0 else 14 + o)
                            kts.append((ktp, ti))
                    na_plan.append((m * 512, 512, kts))
                if with_ctx:
                    na_plan.append((T, C, ctxk))
                for h in range(6):
                    dma("sp", nraw, nab[l, h], writes=["nraw"])
                    op("act", "activation", ["nraw"], ["cbu"], out=cbu, in_=nraw, func=AF.Exp)
                    for ti in range(20):
                        o = ti if ti < 8 else (ti - 8 + 2 if ti < 14 else ti - 14)
                        e0 = 15 - 2 * o
                        op("dve", "tensor_tensor", ["cbu", "rmk"], ["EBh"], out=EBh[:, ti, :], in0=cbu[:, e0 * 64:(e0 + 8) * 64], in1=rmk[:, ti, :], op=ALU.mult)
                    attention(10 + h, 64, NAK[h * 64:(h + 1) * 64, :], NAQ[h * 64:(h + 1) * 64, :],
                              NAV[h], 1.0, na_plan, EBh=EBh)
                S.barrier()
                if stop == (l, "CD"):
                    break

            WA.reset()
            pgs = [WA.alloc([1280], F32) for _ in range(2)]
            e1 = WA.alloc([256], F32)
            ff = WA.alloc([256], F32)
            lf = WA.alloc([256], F32)
            kk = WA.alloc([256], F32)
            Eb = WA.alloc([256], F32)
            Einv = WA.alloc([256], F32)
            e12 = WA.alloc([2, 16], F32)
            EL = WA.alloc([2, 4], F32)
            vbc = [WA.alloc([256], BF16) for _ in range(2)]
            qh = WA.alloc([256], BF16)
            kh = WA.alloc([256], BF16)
            vb = WA.alloc([256], BF16)
            qT = WA.alloc([2, 128], BF16)
            kT = WA.alloc([2, 128], BF16)
            qTc = [WA.alloc([2, 128], BF16) for _ in range(4)]
            for c_ in range(4):
                op("dve", "memset", [], [f"qTc{c_}"], qTc[c_], 0.0)
            ATb = [WA.alloc([128], BF16) for _ in range(2)]
            Sst = [WA.alloc([64], F32) for _ in range(2)]
            Sb = [WA.alloc([64], BF16) for _ in range(2)]
            SEL = WA.alloc([64], F32)
            osum = WA.alloc([256], F32)
            ofl = WA.alloc([256], F32)
            osq = WA.alloc([256], F32)
            ss4 = WA.alloc([4], F32)
            sg = WA.alloc([256], F32)
            yb = WA.alloc([256], BF16)
            yT = WA.alloc([2, 128], BF16)
            ong = WA.alloc([256], F32)
            dma("sp", ong, onormg[l], writes=["ong"])
            for dr in range(2):
                order = [32, 33] + list(range(32)) if dr == 0 else [33, 32] + list(range(31, -1, -1))
                for hp in range(2):
                    op("dve", "memset", [], [f"S{hp}"], Sst[hp], 0.0)
                for ti_, t in enumerate(order):
                    pb2 = ti_ % 2
                    pg, pgk = pgs[pb2], f"pg{pb2}"
                    dma("sp", pg, PHG[t * 128:(t + 1) * 128, :], reads=["PHG"], writes=[pgk])
                    z = pg[:, 512 + 256 * dr:768 + 256 * dr]
                    op("act", "activation", [pgk], ["e1"], out=e1, in_=z, func=AF.Exp, scale=-1.0)
                    op("dve", "tensor_scalar_add", ["e1"], ["e1"], out=e1, in0=e1, scalar1=1.0)
                    op("dve", "reciprocal", ["e1"], ["e1"], out=e1, in_=e1)
                    op("dve", "tensor_tensor", ["e1", "OML"], ["ff"], out=ff, in0=e1, in1=OML[:, dr, l, :], op=ALU.mult)
                    op("dve", "tensor_tensor", ["ff", "LB"], ["ff"], out=ff, in0=ff, in1=LB[:, dr, l, :], op=ALU.add)
                    op("act", "activation", ["ff"], ["lf"], out=lf, in_=ff, func=AF.Ln)
                    op("dve", "tensor_scalar", ["ff"], ["kk"], out=kk, in0=ff, scalar1=-1.0, scalar2=1.0, op0=ALU.mult, op1=ALU.add)
                    pbp, pbpk = P()
                    op("pe", "matmul", ["lf", "consts"], [pbpk], pbp[:, 0:256], lhsT=consts[:, dr, :], rhs=lf, start=True, stop=True)
                    if True:
                        pe12, pe12k = P()
                        for hp in range(2):
                            op("pe", "matmul", ["lf", "sel4"], [pe12k], pe12[:, hp * 16:hp * 16 + 16], lhsT=lf[:, hp * 128:(hp + 1) * 128], rhs=sel4, start=True, stop=True)
                    op("act", "activation", [pbpk], ["Eb"], out=Eb, in_=pbp[:, 0:256], func=AF.Exp)
                    op("act", "activation", [pbpk], ["Einv"], out=Einv, in_=pbp[:, 0:256], func=AF.Exp, scale=-1.0)
                    c1, c2 = (0, 1) if dr == 0 else (2, 3)
                    if True:
                        op("act", "activation", [pe12k], ["e12"], out=e12, in_=pe12[:, 0:32].rearrange("p (a b) -> p a b", a=2), func=AF.Exp)
                        c1, c2 = (0, 1) if dr == 0 else (2, 3)
                        e12v = e12.rearrange("p a (c f) -> p a c f", f=4)
                        op("dve", "tensor_tensor", ["e12"], ["EL"], out=EL, in0=e12v[:, :, :, c1], in1=e12v[:, :, :, c2], op=ALU.mult)
                    op("dve", "scalar_tensor_tensor", [pgk, "Eb"], ["qh"], out=qh, in0=pg[:, 0:256], scalar=0.125, in1=Eb, op0=ALU.mult, op1=ALU.mult)
                    op("dve", "tensor_tensor", ["kk", "Einv"], ["kh"], out=kh, in0=kk, in1=Einv, op=ALU.mult)
                    op("dve", "tensor_copy", [pgk], ["vb"], out=vb, in_=pg[:, 256:512])
                    for (srcb, sk, dstT, dk) in ((qh, "qh", qT, "qT"), (kh, "kh", kT, "kT")):
                        ptt, pttk = P()
                        ptb = ptt[:].bitcast(BF16)
                        for j in range(2):
                            op("pe", "transpose", [sk, "ident"], [pttk], out=ptb[:, j * 128:(j + 1) * 128], in_=srcb[:, j * 128:(j + 1) * 128], identity=ident)
                        op("act", "activation", [pttk], [dk], out=dstT, in_=ptb[:, 0:256].rearrange("p (a b) -> p a b", a=2), func=AF.Copy)
                    for c_ in range(4):
                        op("dve", "tensor_copy", ["qT"], [f"qTc{c_}"], out=qTc[c_][:, :, 32 * c_:32 * c_ + 32], in_=qT[:, :, 32 * c_:32 * c_ + 32])
                    ai = acc_n[0] % 2
                    acc_n[0] += 1
                    po, pok = ps[6 + ai][:], f"ps{6 + ai}"
                    for h in range(4):
                        hp, ro = h // 2, (h % 2) * 64
                        pA, pAk = P()
                        op("pe", "matmul", ["kT", "qT"], [pAk], pA[:, 0:128], lhsT=kT[ro:ro + 64, hp, :], rhs=qT[ro:ro + 64, hp, :], start=True, stop=True)
                        ab = h % 2
                        op("dve", "tensor_tensor", [pAk, "consts"], [f"ATb{ab}"], out=ATb[ab], in0=pA[:, 0:128], in1=consts[:, 2 + dr, :], op=ALU.mult)
                        op("pe", "matmul", [f"ATb{ab}", "vb"], [pok], po[:, h * 64:(h + 1) * 64], lhsT=ATb[ab], rhs=vb[:, h * 64:(h + 1) * 64], start=True, stop=True)
                    op("act", "activation", [pok], ["osum"], out=osum, in_=po[:, 0:256], func=AF.Copy)
                    osum4 = osum.rearrange("p (a b e) -> p a b e", a=2, b=2)
                    corder = (0, 1, 2, 3) if dr == 0 else (3, 2, 1, 0)
                    for ci2, c_ in enumerate(corder):
                        vi = ci2 % 2
                        op("act", "activation", ["vb", "vmask"], [f"vbc{vi}"], out=vbc[vi], in_=vb, func=AF.Copy, scale=vmask[:, c_:c_ + 1])
                        for hp in range(2):
                            op("dve", "tensor_scalar_mul", [f"S{hp}", "e12"], [f"Sb{hp}"], out=Sb[hp], in0=Sst[hp], scalar1=e12[:, hp, 4 * c_ + c1:4 * c_ + c1 + 1])
                            if hp == 0:
                                pR = [P(), P()]
                            for hh in range(2):
                                ro = hh * 64
                                op("pe", "matmul", [f"qTc{c_}", f"Sb{hp}"], [pR[hh][1]], pR[hh][0][:, hp * 64:(hp + 1) * 64], lhsT=qTc[c_][ro:ro + 64, hp, :], rhs=Sb[hp][ro:ro + 64, :],
                                   start=True, stop=True)
                            if hp == 1:
                                for hh in range(2):
                                    op("dve", "tensor_tensor", [pR[hh][1], "osum"], ["osum"], out=osum4[:, :, hh, :], in0=osum4[:, :, hh, :],
                                       in1=pR[hh][0][:, 0:128].rearrange("p (a e) -> p a e", a=2), op=ALU.add)
                            pu, puk = P()
                            op("pe", "matmul", ["kh", f"vbc{vi}"], [puk], pu[:, 0:128], lhsT=kh[:, hp * 128:(hp + 1) * 128], rhs=vbc[vi][:, hp * 128:(hp + 1) * 128], start=True, stop=True)
                            op("dve", "tensor_scalar_mul", [f"S{hp}", "EL", f"Sb{hp}"], ["SEL"], out=SEL, in0=Sst[hp], scalar1=EL[:, hp, c_:c_ + 1])
                            for ro in (0, 64):
                                op("dve", "scalar_tensor_tensor", [puk, "e12", "SEL", f"Sb{hp}"], [f"S{hp}"], out=Sst[hp][ro:ro + 64, :], in0=pu[ro:ro + 64, ro:ro + 64],
                                   scalar=e12[ro:ro + 64, hp, 4 * c_ + c2:4 * c_ + c2 + 1], in1=SEL[ro:ro + 64, :], op0=ALU.mult, op1=ALU.add)
                    if dr == 0:
                        dma("sp", OF[t * 128:(t + 1) * 128, :], osum, reads=["osum"], writes=["OF"])
                    else:
                        dma("sp", ofl, OF[t * 128:(t + 1) * 128, :], reads=["OF"], writes=["ofl"])
                        op("dve", "tensor_tensor", ["osum", "ofl"], ["osum"], out=osum, in0=osum, in1=ofl, op=ALU.add)
                        op("act", "activation", ["osum"], ["osq"], out=osq, in_=osum, func=AF.Square)
                        op("dve", "tensor_reduce", ["osq"], ["ss4"], out=ss4, in_=osq.rearrange("p (h e) -> p h e", h=4), axis=AX.X, op=ALU.add)
                        op("act", "activation", ["ss4", "epsb"], ["ss4"], out=ss4, in_=ss4, func=AF.Ln, scale=1.0 / 64, bias=epsb)
                        op("act", "activation", ["ss4"], ["ss4"], out=ss4, in_=ss4, func=AF.Exp, scale=-0.5)
                        o3 = osum.rearrange("p (h e) -> p h e", h=4)
                        op("dve", "tensor_tensor", ["osum", "ss4"], ["osum"], out=o3, in0=o3, in1=ss4.unsqueeze(2).to_broadcast([128, 4, 64]), op=ALU.mult)
                        op("dve", "tensor_tensor", ["osum", "ong"], ["osum"], out=osum, in0=osum, in1=ong, op=ALU.mult)
                        g_ = pg[:, 1024:1280]
                        op("act", "activation", [pgk], ["sg"], out=sg, in_=g_, func=AF.Exp, scale=-1.0)
                        op("dve", "tensor_scalar_add", ["sg"], ["sg"], out=sg, in0=sg, scalar1=1.0)
                        op("dve", "reciprocal", ["sg"], ["sg"], out=sg, in_=sg)
                        op("dve", "tensor_tensor", ["sg", pgk], ["sg"], out=sg, in0=sg, in1=g_, op=ALU.mult)
                        op("dve", "tensor_tensor", ["sg", "osum"], ["yb"], out=yb, in0=osum, in1=sg, op=ALU.mult)
                        ptt, pttk = P()
                        ptb = ptt[:].bitcast(BF16)
                        for j in range(2):
                            op("pe", "transpose", ["yb", "ident"], [pttk], out=ptb[:, j * 128:(j + 1) * 128], in_=yb[:, j * 128:(j + 1) * 128], identity=ident)
                        op("act", "activation", [pttk], ["yT"], out=yT, in_=ptb[:, 0:256].rearrange("p (a b) -> p a b", a=2), func=AF.Copy)
                        for h in range(4):
                            ro = (h % 2) * 64
                            dma("sp", CAT[h, :, t * 128:(t + 1) * 128], yT[ro:ro + 64, h // 2, :], reads=["yT"], writes=["CAT"])
            S.barrier()
            if stop == (l, "E"):
                break

            NT_ = NTILE if with_ctx else 32
            WA.reset()
            h2tm = WA.alloc([NTILE, D], BF16)
            AFFs = WA.alloc([NTILE, 16], F32)
            GF = [WA.alloc([D], F32) for _ in range(2)]
            markF = WA.off
            woutb = WA.alloc([16, D], BF16, parts=64)
            rwb = WA.alloc([8, 16], BF16)
            for s4 in range(4):
                dma("pool", woutb[:, s4 * 4:(s4 + 1) * 4, :], w_out[l, s4 * 256:(s4 + 1) * 256, :].rearrange("(s d) j -> d s j", d=64), writes=["woutb"])
            dma("pool", rwb, router_w[l].rearrange("(k p) e -> p k e", p=128), writes=["rwb"])
            GA = [WA.alloc([D], F32) for _ in range(2)]
            AFb = [WA.alloc([D], F32) for _ in range(2)]
            BFb = [WA.alloc([D], F32) for _ in range(2)]
            dg = WA.alloc([128], F32)

            def bcast_rows(dst, key, col_ap_fn, rk):
                for half in range(2):
                    pt, pk = P()
                    for k4 in range(4):
                        kt = half * 4 + k4
                        op("dve", "tensor_scalar_mul", ["identf"] + rk, ["dg"], out=dg, in0=identf, scalar1=col_ap_fn(kt))
                        op("pe", "matmul", ["dg", "onesf"], [pk], pt[:, k4 * 128:(k4 + 1) * 128], lhsT=onesf, rhs=dg, start=True, stop=True)
                    op("act", "activation", [pk], [key], out=dst[:, half * 512:(half + 1) * 512], in_=pt, func=AF.Copy)

            for w_ in range(2):
                bcast_rows(GA[w_], f"GA{w_}", lambda kt, w_=w_: MOD[:, l, 16 + kt, w_:w_ + 1], ["MOD"])
                bcast_rows(GF[w_], f"GF{w_}", lambda kt, w_=w_: MOD[:, l, 40 + kt, w_:w_ + 1], ["MOD"])
                bcast_rows(AFb[w_], f"AFb{w_}", lambda kt, w_=w_: AB[:, l, 2, kt, w_:w_ + 1], ["AB"])
                bcast_rows(BFb[w_], f"BFb{w_}", lambda kt, w_=w_: AB[:, l, 3, kt, w_:w_ + 1], ["AB"])
            xts = [WA.alloc([D], F32) for _ in range(2)]
            catt = [WA.alloc([16, 128], BF16, parts=64) for _ in range(2)]
            scratch = (WA.alloc([D], F32), WA.alloc([1], F32), WA.alloc([1], F32), WA.alloc([D], BF16), WA.alloc([8, 128], F32))
            h2T = WA.alloc([8, 128], BF16)
            tmpx = WA.alloc([D], F32)
            mx = WA.alloc([1], F32)
            sme = WA.alloc([1], F32)
            ex = WA.alloc([16], F32)
            for t in range(NT_):
                b = t % 2
                w_ = 0 if t < 32 else 1
                for s4 in range(4):
                    dma("sp", catt[b][:, s4 * 4:(s4 + 1) * 4, :], CAT[s4 * 4:(s4 + 1) * 4, :, t * 128:(t + 1) * 128].rearrange("s d n -> d s n"), reads=["CAT"], writes=[f"catt{b}"])
                dma("sp", xts[b], XS[t * 128:(t + 1) * 128, :], reads=["XS"], writes=[f"xt{b}"])
                for jc in range(2):
                    pt, pk = P()
                    for s_ in range(16):
                        op("pe", "matmul", [f"catt{b}", "woutb"], [pk], pt, lhsT=catt[b][:, s_, :], rhs=woutb[:, s_, jc * 512:(jc + 1) * 512], start=(s_ == 0), stop=(s_ == 15))
                    op("dve", "tensor_tensor", [pk, f"GA{w_}"], ["tmpx"], out=tmpx[:, jc * 512:(jc + 1) * 512], in0=pt, in1=GA[w_][:, jc * 512:(jc + 1) * 512], op=ALU.mult)
                op("dve", "tensor_tensor", ["tmpx", f"xt{b}"], [f"xt{b}"], out=xts[b], in0=xts[b], in1=tmpx, op=ALU.add)
                dma("sp", XS[t * 128:(t + 1) * 128, :], xts[b], reads=[f"xt{b}"], writes=["XS"])
                norm_tile(xts[b], f"xt{b}", l, 2, w_, h2T, "h2T", scratch)
                xn = scratch[3]
                op("dve", "tensor_tensor", ["n_xn", f"AFb{w_}"], ["tmpx"], out=tmpx, in0=xn, in1=AFb[w_], op=ALU.mult)
                op("dve", "tensor_tensor", ["tmpx", f"BFb{w_}"], [f"h2tm{t}"], out=h2tm[:, t, :], in0=tmpx, in1=BFb[w_], op=ALU.add)
                pt, pk = P()
                for kt in range(8):
                    op("pe", "matmul", ["h2T", "rwb"], [pk], pt[:, 0:16], lhsT=h2T[:, kt, :], rhs=rwb[:, kt, :], start=(kt == 0), stop=(kt == 7))
                op("dve", "reduce_max", [pk], ["mx"], out=mx, in_=pt[:, 0:16], axis=AX.X)
                op("dve", "tensor_scalar_mul", ["mx"], ["mx"], out=mx, in0=mx, scalar1=-1.0)
                op("act", "activation", [pk, "mx"], ["ex", "sme"], out=ex, in_=pt[:, 0:16], func=AF.Exp, bias=mx, accum_out=sme)
                op("dve", "reciprocal", ["sme"], ["sme"], out=sme, in_=sme)
                op("dve", "tensor_scalar_mul", ["ex", "sme"], ["AFF"], out=AFFs[:, t, :], in0=ex, scalar1=sme)
            S.barrier()
            if stop == (l, "F"):
                break

            WA.off = markF
            Mt = WA.alloc([NTILE, 16], BF16)
            Mf = WA.alloc([NTILE, 16], F32)
            RK = WA.alloc([NTILE, 16], F32)
            INFO = WA.alloc([NTILE, 16, 5], BF16)
            tki = WA.alloc([NTILE, 2], F32)
            iot = WA.alloc([128], F32)
            lo = WA.alloc([16], F32)
            hi = WA.alloc([16], F32)
            mid = WA.alloc([16], F32)
            cntb = WA.alloc([16], BF16)
            cntf = WA.alloc([16], F32)
            gef = WA.alloc([16], F32)
            d1 = WA.alloc([16], F32)
            ghi = WA.alloc([NTILE, 16], BF16)
            gl32 = WA.alloc([NTILE, 16], F32)
            dma("sp", tki, tokinfo_d, writes=["tki"])
            dma("sp", iot, iota_d, writes=["iot"])
            groups = [(0, 32, 511.5)] + ([(32, 34, 31.5)] if with_ctx else [])
            for (t0, t1_, capm) in groups:
                nt_ = t1_ - t0
                A_ = AFFs[:, t0:t1_, :]
                op("dve", "memset", [], ["lo"], lo, 0.0)
                op("dve", "memset", [], ["hi"], hi, 1.0)
                for it in range(26):
                    op("dve", "tensor_tensor", ["lo", "hi"], ["mid"], out=mid, in0=lo, in1=hi, op=ALU.add)
                    op("dve", "tensor_scalar_mul", ["mid"], ["mid"], out=mid, in0=mid, scalar1=0.5)
                    op("dve", "tensor_tensor", ["AFF", "mid"], ["Mt"], out=Mt[:, t0:t1_, :], in0=A_, in1=mid.unsqueeze(1).to_broadcast([128, nt_, 16]), op=ALU.is_ge)
                    op("dve", "tensor_reduce", ["Mt"], ["cntf"], out=cntf, in_=Mt[:, t0:t1_, :].rearrange("p t e -> p e t"), axis=AX.X, op=ALU.add)
                    op("dve", "tensor_copy", ["cntf"], ["cntb"], out=cntb, in_=cntf)
                    pt, pk = P()
                    op("pe", "matmul", ["cntb", "onesb"], [pk], pt[:, 0:16], lhsT=onesb, rhs=cntb, start=True, stop=True)
                    op("dve", "tensor_single_scalar", [pk], ["gef"], out=gef, in_=pt[:, 0:16], scalar=capm, op=ALU.is_ge)
                    op("dve", "tensor_tensor", ["mid", "lo"], ["d1"], out=d1, in0=mid, in1=lo, op=ALU.subtract)
                    op("dve", "tensor_tensor", ["d1", "gef"], ["d1"], out=d1, in0=d1, in1=gef, op=ALU.mult)
                    op("dve", "tensor_tensor", ["d1", "lo"], ["lo"], out=lo, in0=lo, in1=d1, op=ALU.add)
                    op("dve", "tensor_tensor", ["mid", "hi"], ["d1"], out=d1, in0=hi, in1=mid, op=ALU.subtract)
                    op("dve", "tensor_tensor", ["d1", "gef"], ["d1"], out=d1, in0=d1, in1=gef, op=ALU.mult)
                    op("dve", "tensor_tensor", ["d1", "mid"], ["hi"], out=hi, in0=mid, in1=d1, op=ALU.add)
                op("dve", "tensor_tensor", ["AFF", "lo"], ["Mf"], out=Mf[:, t0:t1_, :], in0=A_, in1=lo.unsqueeze(1).to_broadcast([128, nt_, 16]), op=ALU.is_ge)
            op("dve", "tensor_copy", ["Mf"], ["Mt"], out=Mt[:, 0:NT_, :], in_=Mf[:, 0:NT_, :])
            segs = [list(range(sg * 4, sg * 4 + 4)) for sg in range(8)] + ([[32, 33]] if with_ctx else [])
            for tiles in segs:
                for ji, t in enumerate(tiles):
                    pt, pk = P()
                    op("pe", "matmul", ["Mt", "maskb"], [pk], pt[:, 0:16], lhsT=maskb[:, 0, :], rhs=Mt[:, t, :], start=True, stop=(ji == 0))
                    for jj in range(ji):
                        op("pe", "matmul", ["Mt", "onesb"], [pk], pt[:, 0:16], lhsT=onesb, rhs=Mt[:, tiles[jj], :], start=False, stop=(jj == ji - 1))
                    op("dve", "tensor_scalar_add", [pk], ["RK"], out=RK[:, t, :], in0=pt[:, 0:16], scalar1=-1.0)
            op("dve", "memset", [], ["INFO"], INFO, 1.0)
            op("dve", "tensor_copy", ["tki"], ["INFO"], out=INFO[:, :, :, 0:2], in_=tki.unsqueeze(2).to_broadcast([128, NTILE, 16, 2]))
            op("dve", "tensor_copy", ["AFF"], ["ghi"], out=ghi[:, 0:NT_, :], in_=AFFs[:, 0:NT_, :])
            op("dve", "tensor_copy", ["ghi"], ["INFO"], out=INFO[:, 0:NT_, :, 3], in_=ghi[:, 0:NT_, :])
            op("dve", "tensor_tensor", ["AFF", "ghi"], ["gl32"], out=gl32[:, 0:NT_, :], in0=AFFs[:, 0:NT_, :], in1=ghi[:, 0:NT_, :], op=ALU.subtract)
            op("dve", "tensor_copy", ["gl32"], ["INFO"], out=INFO[:, 0:NT_, :, 4], in_=gl32[:, 0:NT_, :])

            w1b = WA.alloc([8, D], BF16)
            w3b = WA.alloc([8, D], BF16)
            w2b = WA.alloc([8, D], BF16)
            NSLOT = 128 * len(segs)
            Sel = WA.alloc([NTILE, 128], BF16)
            xsT = WA.alloc([8, 512], BF16)
            actT = WA.alloc([8, 512], BF16)
            sa = WA.alloc([512], F32)
            yo = [y_[:, :] for y_ in yo_t]
            sinfo = WA.alloc([9, 5], F32)
            idxf = WA.alloc([9, 1], F32)
            idxi = [i_[:, :, :] for i_ in idx_t]
            gate = WA.alloc([9], F32)
            pcol = WA.alloc([1], F32)
            op("dve", "scalar_tensor_tensor", ["tki"], ["pcol"], out=pcol, in0=tki[:, 0, 0:1], scalar=64.0, in1=tki[:, 0, 1:2], op0=ALU.mult, op1=ALU.add)
            op("dve", "tensor_scalar_add", ["pcol"], ["pcol"], out=pcol, in0=pcol, scalar1=float(TT))
            yn = [0]
            for e_ in range(16):
                for (wb_, wsrc_, wk_) in ((w1b, exp_w1, "w1b"), (w3b, exp_w3, "w3b"), (w2b, exp_w2, "w2b")):
                    for kt_ in range(8):
                        dma("pool", wb_[:, kt_, :], wsrc_[l, e_, kt_ * 128:(kt_ + 1) * 128, :], writes=[wk_])
                for t in range(NT_):
                    op("dve", "tensor_scalar", ["iot", "RK", "Mf"], [f"Sel{t}"], out=Sel[:, t, :], in0=iot, scalar1=RK[:, t, e_:e_ + 1], scalar2=Mf[:, t, e_:e_ + 1],
                       op0=ALU.is_equal, op1=ALU.mult)
                ib = e_ % 2
                fchunks = [(0, 4), (4, 8)] + ([(8, 9)] if with_ctx else [])
                for (sg0, sg1) in fchunks:
                    s0, sn = 0, (sg1 - sg0) * 128
                    for sg in range(sg0, sg1):
                        tiles = segs[sg]
                        so = (sg - sg0) * 128
                        skeys = [f"Sel{t}" for t in tiles]
                        hkeys2 = [f"h2tm{t}" for t in tiles]
                        for kh_ in range(2):
                            pt, pk = P()
                            for k4 in range(4):
                                kt = kh_ * 4 + k4
                                for ji, t in enumerate(tiles):
                                    op("pe", "matmul", skeys + hkeys2, [pk], pt[:, k4 * 128:(k4 + 1) * 128], lhsT=h2tm[:, t, kt * 128:(kt + 1) * 128], rhs=Sel[:, t, :],
                                       start=(ji == 0), stop=(ji == len(tiles) - 1))
                            evac(pk, xsT[:, kh_ * 4:(kh_ + 1) * 4, so:so + 128], "xsT", pt.rearrange("p (a b) -> p a b", a=4))
                        pt, pk = P()
                        for ji, t in enumerate(tiles):
                            op("pe", "matmul", skeys + ["INFO"], [pk], pt[:, 0:5], lhsT=Sel[:, t, :], rhs=INFO[:, t, e_, :], start=(ji == 0), stop=(ji == len(tiles) - 1))
                        op("dve", "tensor_copy", [pk], ["sinfo"], out=sinfo[:, sg, :], in_=pt[:, 0:5])
                    op("dve", "scalar_tensor_tensor", ["sinfo"], ["idxf"], out=idxf[:, sg0:sg1, :], in0=sinfo[:, sg0:sg1, 0:1], scalar=64.0, in1=sinfo[:, sg0:sg1, 1:2], op0=ALU.mult, op1=ALU.add)
                    op("dve", "tensor_scalar", ["sinfo"], ["gate"], out=gate[:, sg0:sg1], in0=sinfo[:, sg0:sg1, 2], scalar1=-1.0, scalar2=1.0, op0=ALU.mult, op1=ALU.add)
                    op("dve", "scalar_tensor_tensor", ["idxf", "gate", "pcol"], ["idxf"], out=idxf[:, sg0:sg1, 0], in0=gate[:, sg0:sg1], scalar=pcol[:, 0:1], in1=idxf[:, sg0:sg1, 0], op0=ALU.mult, op1=ALU.add)
                    if sg0 == 8:
                        op("dve", "scalar_tensor_tensor", ["idxf", "sinfo"], ["idxf"], out=idxf[:, sg0:sg1, 0], in0=sinfo[:, sg0:sg1, 2], scalar=float(T), in1=idxf[:, sg0:sg1, 0], op0=ALU.mult, op1=ALU.add)
                    op("dve", "tensor_copy", ["idxf"], [f"idxi{ib}"], out=idxi[ib][:, sg0:sg1, :], in_=idxf[:, sg0:sg1, :])
                    op("dve", "tensor_tensor", ["sinfo", "idxf"], ["gate"], out=gate[:, sg0:sg1], in0=sinfo[:, sg0:sg1, 3], in1=sinfo[:, sg0:sg1, 4], op=ALU.add)
                    for ft in range(8):
                        pa, pak = P()
                        pu, puk = P()
                        for (pp, kk_, wb_, wk_) in ((pa, pak, w1b, "w1b"), (pu, puk, w3b, "w3b")):
                            for kt in range(8):
                                op("pe", "matmul", ["xsT", wk_], [kk_], pp[:, 0:sn], lhsT=wb_[:, kt, ft * 128:(ft + 1) * 128], rhs=xsT[:, kt, s0:s0 + sn], start=(kt == 0), stop=(kt == 7))
                        op("act", "activation", [pak], ["sa"], out=sa[:, 0:sn], in_=pa[:, 0:sn], func=AF.Silu)
                        op("dve", "tensor_tensor", [puk, "sa"], ["actT"], out=actT[:, ft, 0:sn], in0=pu[:, 0:sn], in1=sa[:, 0:sn], op=ALU.mult)
                    for st in range(sn // 128):
                        sg = sg0 + st
                        w_ = 0 if sg < 8 else 1
                        yb_ = yn[0] % 2
                        yn[0] += 1
                        for jc in range(2):
                            py, pyk = P()
                            for ft in range(8):
                                op("pe", "matmul", ["actT", "w2b"], [pyk], py, lhsT=actT[:, ft, st * 128:(st + 1) * 128], rhs=w2b[:, ft, jc * 512:(jc + 1) * 512], start=(ft == 0), stop=(ft == 7))
                            op("dve", "scalar_tensor_tensor", [pyk, "gate", f"GF{w_}"], [f"yo{yb_}"], out=yo[yb_][:, jc * 512:(jc + 1) * 512], in0=py, scalar=gate[:, sg:sg + 1],
                               in1=GF[w_][:, jc * 512:(jc + 1) * 512], op0=ALU.mult, op1=ALU.mult)
                        S.add("pool", lambda e, s, yb_=yb_, ib=ib, sg=sg: e.indirect_dma_start(
                            out=XS_h[:, :], out_offset=bass.IndirectOffsetOnAxis(ap=idx_t[ib][:, sg, :], axis=0), in_=yo_t[yb_][:, :], in_offset=None,
                            bounds_check=regs["bc"], oob_is_err=False, compute_op=ALU.add).then_inc(s, 16),
                            reads=[f"yo{yb_}", f"idxi{ib}"] + [("sc", (e_ + 1) % 2, q_) for q_ in range(9)], writes=[("sc", e_ % 2, sg), "XSsc"] if False else [("sc", e_ % 2, sg)], dma=True)
            S.barrier()
            if stop == (l, "H"):
                break

        if stop is None:
            WA.reset()
            fg = WA.alloc([D], F32)
            dma("sp", fg, fng, writes=["fg"])
            xts = [WA.alloc([D], F32) for _ in range(2)]
            sq_ = WA.alloc([D], F32)
            ssq_ = WA.alloc([1], F32)
            yo2 = [WA.alloc([D], F32) for _ in range(2)]
            for t in range(32):
                b = t % 2
                dma("sp", xts[b], XS[t * 128:(t + 1) * 128, :], reads=["XS"], writes=[f"fx{b}"])
                op("act", "activation", [f"fx{b}"], ["fsq", "fssq"], out=sq_, in_=xts[b], func=AF.Square, accum_out=ssq_)
                op("act", "activation", ["fssq", "epsb"], ["fssq"], out=ssq_, in_=ssq_, func=AF.Ln, scale=1.0 / D, bias=epsb)
                op("act", "activation", ["fssq"], ["fssq"], out=ssq_, in_=ssq_, func=AF.Exp, scale=-0.5)
                op("dve", "scalar_tensor_tensor", [f"fx{b}", "fssq", "fg"], [f"fy{b}"], out=yo2[b], in0=xts[b], scalar=ssq_, in1=fg, op0=ALU.mult, op1=ALU.mult)
                dma("sp", out[t * 128:(t + 1) * 128, :], yo2[b], reads=[f"fy{b}"], writes=["out"])

        S.barrier()
        final_ops = [i for i, o in enumerate(S.ops) if o.dma][-60:]
        S.emit(final_wait_ops=final_ops)
    return nc


def _col(v):
    v = np.asarray(v, np.float32)
    return np.ascontiguousarray(np.swapaxes(v.reshape(v.shape[:-1] + (v.shape[-1] // 128, 128)), -1, -2))


def _constants():
    c = {}
    c["ident"] = np.eye(128, dtype=np.float32).astype(ml_dtypes.bfloat16)
    c["identf"] = np.eye(128, dtype=np.float32)
    j = np.arange(128)[:, None]
    i = np.arange(128)[None, :]
    consts = np.zeros((128, 5, 128), np.float32)
    consts[:, 4] = (j <= i)
    same = (j // 32) == (i // 32)
    midf = (i // 32) * 32 + 15
    midb = (i // 32) * 32 + 16
    consts[:, 0] = same * ((j <= i).astype(np.float32) - (j <= midf).astype(np.float32))
    consts[:, 1] = same * ((j >= i).astype(np.float32) - (j >= midb).astype(np.float32))
    consts[:, 2] = same & (j <= i)
    consts[:, 3] = same & (j >= i)
    c["consts"] = consts
    sel = np.zeros((128, 16), np.float32)
    vm = np.zeros((128, 4), np.float32)
    jj = np.arange(128)
    for cc in range(4):
        inb = (jj // 32) == cc
        sel[:, 4 * cc + 0] = inb & (jj <= 32 * cc + 15)
        sel[:, 4 * cc + 1] = inb & (jj > 32 * cc + 15)
        sel[:, 4 * cc + 2] = inb & (jj >= 32 * cc + 16)
        sel[:, 4 * cc + 3] = inb & (jj < 32 * cc + 16)
        vm[:, cc] = inb
    c["sel4"] = sel
    c["vmask"] = vm
    idx = (np.arange(NTILE)[None, :] * 128 + np.arange(128)[:, None])
    idx = np.where(idx >= T, idx - T, idx)
    c["tokinfo"] = np.stack([idx // 64, idx % 64], -1).astype(np.float32)
    c["iota"] = np.broadcast_to(np.arange(128, dtype=np.float32)[None, :], (128, 128)).copy()
    t = np.arange(T)
    p = np.arange(32)
    within = p % 16
    fi = within % 8
    inv = (np.float32(10000.0) ** (-(np.arange(0, 16, 2, dtype=np.float32)) / np.float32(16.0))).astype(np.float32)
    pos = np.where((p // 16)[:, None] == 0, (t // 64)[None, :], (t % 64)[None, :]).astype(np.float32)
    ang = (pos * inv[fi][:, None]).astype(np.float32)
    rope = np.zeros((2, 32, TT), np.float32)
    rope[0, :, :T] = np.cos(ang)
    rope[1, :, :T] = np.sin(ang) * np.where(within < 8, -1.0, 1.0)[:, None]
    rope[0, :, T:] = 1.0
    c["rope"] = rope
    rm = np.zeros((128, 20, 512), np.float32)
    classes = [(3, o) for o in range(8)] + [(0, o) for o in range(2, 8)] + [(7, o) for o in range(6)]
    pp = np.arange(128)
    for ti, (m, o) in enumerate(classes):
        a = 8 * m - 4 + 2 * o
        kr = a + (pp >= 64)
        for jj in range(8):
            qr = 8 * m + jj
            rs = min(max(qr - 4, 0), 56)
            ok = (kr >= rs) & (kr <= rs + 7)
            rm[:, ti, jj * 64:(jj + 1) * 64] = ok[:, None]
    c["rowmask"] = rm.astype(ml_dtypes.bfloat16)
    return c


ROPE_PARTNER = np.array([(p + 8) if (p % 16) < 8 else (p - 8) for p in range(32)])


def _shared_inputs(inp):
    f = lambda k: np.asarray(inp[k], np.float32)
    sh = dict(_constants())
    sh["ada_w"] = f("ada_w")
    sh["ada_bT"] = np.ascontiguousarray(f("ada_b").reshape(DEPTH, 48, 128).transpose(0, 2, 1))
    sh["gmixT"] = _col(f("norm_mix_g"))
    sh["gffnT"] = _col(f("norm_ffn_g"))
    w_in = f("w_in")
    sh["w_in"] = np.ascontiguousarray(np.concatenate([w_in, w_in[:, :, 1664 + ROPE_PARTNER]], axis=-1))
    sh["lbl"] = np.ascontiguousarray(np.broadcast_to(f("hgrn_lb_logits")[None], (128, 2, DEPTH, 256)))
    sh["onormg"] = np.ascontiguousarray(np.broadcast_to(np.tile(f("hgrn_onorm_g"), (1, 4))[:, None, :], (DEPTH, 128, 256)))
    sh["qng"] = _col(f("mla_qnorm_g"))
    sh["kvng"] = _col(f("mla_kvnorm_g"))
    w_uq = f("mla_w_uq")
    perm = np.arange(576).reshape(6, 96)
    perm2 = perm.copy()
    perm2[:, 64:] = perm[:, 64:][:, ROPE_PARTNER]
    sh["w_uq"] = np.ascontiguousarray(np.concatenate([w_uq, w_uq[:, :, perm2.reshape(-1)]], axis=-1))
    sh["w_ukv"] = f("mla_w_ukv")
    rpb = f("na_rpb")
    ei = np.arange(24)
    e = ei - 4
    cq = np.arange(64)
    p = np.arange(128)
    ck = p % 64
    d = np.where(p[:, None] < 64, 14 - e[None, :], 15 - e[None, :])
    cs = np.clip(cq - 8, 0, 48)
    colok = (ck[:, None] >= cs[None, :]) & (ck[:, None] <= cs[None, :] + 15)
    dcol = ck[:, None] - cq[None, :] + 15
    dok = (d >= 0) & (d <= 14)
    dcl = np.clip(d, 0, 14)
    dcolc = np.clip(dcol, 0, 30)
    g = rpb[:, :, dcl[:, :, None], dcolc[:, None, :]]
    okk = dok[:, :, None] & colok[:, None, :]
    nab = np.where(okk[None, None], g, np.float32(-30000.0)).astype(np.float32)
    sh["nab"] = np.ascontiguousarray(nab.reshape(DEPTH, 6, 128, 1536))
    sh["w_out"] = f("w_out")
    sh["router_w"] = f("router_w")
    sh["exp_w1"] = f("exp_w1")
    sh["exp_w3"] = f("exp_w3")
    sh["exp_w2"] = f("exp_w2")
    sh["fng"] = np.ascontiguousarray(np.broadcast_to(f("final_norm_g")[None, :], (128, D)))
    return sh


def _core_inputs(inp, sh, b):
    m = dict(sh)
    m["x"] = np.ascontiguousarray(np.asarray(inp["x"][b], np.float32))
    m["ctx"] = np.ascontiguousarray(np.asarray(inp["ctx"][b], np.float32))
    cc = np.stack([_col(np.asarray(inp["c"][b], np.float32)), _col(np.asarray(inp["c_ctx"], np.float32))], -1)
    m["ccol"] = np.ascontiguousarray(cc)
    return m


def kernel(**inputs):
    nc = build()
    sh = _shared_inputs(inputs)
    in_maps = [_core_inputs(inputs, sh, b) for b in range(8)]
    res = run_bass_kernel_spmd(nc, in_maps, core_ids=list(range(8)))
    return np.stack([np.asarray(r["out"], np.float32) for r in res.results], axis=0)
```
